# Optimizing a Trainium2 kernel written in Bass

```python
import math
import jax, jax.numpy as jnp
from jax import lax
import numpy as np

D_MODEL = 1024
BATCH = 4
SEQ = 4096
DEPTH = 4

HEAD_DIM = 64
NSA_HEADS = 4
NSA_KV_HEADS = 1
CMP_LEN = 32
CMP_STRIDE = 16
CMP_HIDDEN = 128
SEL_BLOCK = 64
SEL_TOP = 16
NSA_WINDOW = 512
MLA_HEADS = 4
MLA_Q_RANK = 256
MLA_KV_RANK = 128
MLA_NOPE = 64
MLA_ROPE = 32
MLA_V = 64
ROPE_THETA = 10000.0
SWA_HEADS = 8
SWA_KV_HEADS = 1
SWA_WINDOW = 128
REL_BUCKETS = 32
REL_MAX_DIST = 512
N_BIAS_HEADS = NSA_HEADS + SWA_HEADS

Q_BLOCK = 128
NORM_EPS = 1e-6
NEG = -1e30
BIG = 1e9

W_A = NSA_HEADS * HEAD_DIM
W_B = MLA_HEADS * MLA_V
W_C = SWA_HEADS * HEAD_DIM
D_MIX = W_A + W_B + W_C

A_Q = NSA_HEADS * HEAD_DIM
A_KV = 6 * NSA_KV_HEADS * HEAD_DIM
A_GATE = 3 * NSA_HEADS
B_CQ = MLA_Q_RANK
B_CKV = MLA_KV_RANK
B_KR = MLA_ROPE
C_Q = SWA_HEADS * HEAD_DIM
C_KV = 2 * SWA_KV_HEADS * HEAD_DIM
IN_SIZES = (A_Q, A_KV, A_GATE, B_CQ, B_CKV, B_KR, C_Q, C_KV, D_MIX)
D_IN = A_Q + A_KV + A_GATE + B_CQ + B_CKV + B_KR + C_Q + C_KV + D_MIX

kernel_name = 'hymba_nsa_mla_swa_sandwich'


def rms_norm(x, g):
    xf = x.astype(jnp.float32)
    y = xf * lax.rsqrt(jnp.mean(xf * xf, axis=-1, keepdims=True) + NORM_EPS)
    return (y * g.astype(jnp.float32)).astype(x.dtype)


def split_cols(h):
    outs, o = [], 0
    for s in IN_SIZES:
        outs.append(h[..., o:o + s])
        o += s
    return outs


def t5_bucket(dist):
    dist = jnp.maximum(dist, 0)
    exact = REL_BUCKETS // 2
    large = exact + (jnp.log(jnp.maximum(dist, 1).astype(jnp.float32) / exact)
                     / math.log(REL_MAX_DIST / exact) * (REL_BUCKETS - exact)).astype(jnp.int32)
    large = jnp.minimum(large, REL_BUCKETS - 1)
    return jnp.where(dist < exact, dist, large)


def rope(x, pos):
    half = x.shape[-1] // 2
    inv = ROPE_THETA ** (-jnp.arange(half, dtype=jnp.float32) / half)
    ang = pos.astype(jnp.float32)[:, None] * inv[None, :]
    cos, sin = jnp.cos(ang)[:, None, :], jnp.sin(ang)[:, None, :]
    x1, x2 = x[..., :half].astype(jnp.float32), x[..., half:].astype(jnp.float32)
    return jnp.concatenate([x1 * cos - x2 * sin, x2 * cos + x1 * sin], axis=-1).astype(x.dtype)


def banded_attention(q, k, v, window, bias_heads, sinks):
    B, T, H, dh = q.shape
    G = k.shape[2]
    hpg = H // G
    nq = T // Q_BLOCK
    nb = window // Q_BLOCK
    K = (nb + 1) * Q_BLOCK
    pad = ((0, 0), (nb * Q_BLOCK, 0), (0, 0), (0, 0))

    def band(t):
        tp = jnp.pad(t, pad).reshape(B, nq + nb, Q_BLOCK, G, dh)
        return jnp.concatenate([tp[:, i:i + nq] for i in range(nb + 1)], axis=2)

    kb, vb = band(k), band(v)
    qb = q.reshape(B, nq, Q_BLOCK, G, hpg, dh)
    logits = jnp.einsum('bnqghd,bnkgd->bnghqk', qb, kb).astype(jnp.float32) * (dh ** -0.5)
    i = jnp.arange(Q_BLOCK)[:, None]
    j = jnp.arange(K)[None, :]
    dist = nb * Q_BLOCK + i - j
    kpos = (jnp.arange(nq)[:, None, None] - nb) * Q_BLOCK + j[None]
    mask = (dist >= 0) & (dist < window) & (kpos >= 0)
    bias = bias_heads[:, t5_bucket(dist)].reshape(G, hpg, Q_BLOCK, K)
    logits = jnp.where(mask[None, :, None, None], logits + bias, NEG)
    if sinks is None:
        p = jax.nn.softmax(logits, axis=-1)
    else:
        s = jnp.broadcast_to(sinks.reshape(G, hpg, 1, 1).astype(jnp.float32), logits.shape[:-1] + (1,))
        p = jax.nn.softmax(jnp.concatenate([logits, s], axis=-1), axis=-1)[..., :-1]
    out = jnp.einsum('bnghqk,bnkgd->bnqghd', p.astype(v.dtype), vb)
    return out.reshape(B, T, H, dh)


def causal_block_attention(q, k, v, scale):
    B, T, H, dk = q.shape
    nq = T // Q_BLOCK
    qb = q.reshape(B, nq, Q_BLOCK, H, dk).transpose(1, 0, 2, 3, 4)
    kpos = jnp.arange(T)

    def one(args):
        qc, n = args
        logits = jnp.einsum('bqhd,bkhd->bhqk', qc, k).astype(jnp.float32) * scale
        qpos = n * Q_BLOCK + jnp.arange(Q_BLOCK)
        mask = kpos[None, :] <= qpos[:, None]
        p = jax.nn.softmax(jnp.where(mask, logits, NEG), axis=-1)
        return jnp.einsum('bhqk,bkhd->bqhd', p.astype(v.dtype), v)

    out = lax.map(one, (qb, jnp.arange(nq)))
    return out.transpose(1, 0, 2, 3, 4).reshape(B, T, H, v.shape[-1])


def nsa_attention(q, kv, gates, cmp_pos, cmp_w1, cmp_w2, bias_heads):
    B, T, H, dh = q.shape
    G = kv.shape[3]
    hpg = H // G
    scale = dh ** -0.5
    k_cmp, v_cmp, k_slc, v_slc, k_win, v_win = [kv[:, :, i] for i in range(6)]
    pos = jnp.arange(T)
    qg = q.reshape(B, T, G, hpg, dh)

    nc = T // CMP_STRIDE - 1

    def compress(t, pe, w1, w2):
        ch = t.reshape(B, T // CMP_STRIDE, CMP_STRIDE, G, dh)
        blk = jnp.concatenate([ch[:, :-1], ch[:, 1:]], axis=2) + pe[:, None, :]
        blk = blk.transpose(0, 1, 3, 2, 4).reshape(B, nc, G, CMP_LEN * dh)
        return jax.nn.silu(blk @ w1) @ w2

    kc = compress(k_cmp, cmp_pos[0], cmp_w1[0], cmp_w2[0])
    vc = compress(v_cmp, cmp_pos[1], cmp_w1[1], cmp_w2[1])
    lc = jnp.einsum('btghd,bcgd->bghtc', qg, kc).astype(jnp.float32) * scale
    cend = jnp.arange(nc) * CMP_STRIDE + CMP_LEN - 1
    dist_c = pos[:, None] - cend[None, :]
    valid_c = dist_c >= 0
    lc = lc + bias_heads[:, t5_bucket(dist_c)].reshape(G, hpg, T, nc)
    p_c = jax.nn.softmax(jnp.where(valid_c, lc, NEG), axis=-1) * valid_c
    o_cmp = jnp.einsum('bghtc,bcgd->btghd', p_c.astype(vc.dtype), vc)

    ns = T // SEL_BLOCK
    n_top = min(SEL_TOP, ns)
    sstart = np.arange(ns) * SEL_BLOCK
    cstart = np.arange(nc) * CMP_STRIDE
    overlap = (np.clip(np.minimum(cstart[:, None] + CMP_LEN, sstart[None, :] + SEL_BLOCK)
                       - np.maximum(cstart[:, None], sstart[None, :]), 0, None) / CMP_STRIDE).astype(np.float32)
    imp = jnp.einsum('bghtc,cs->bgts', p_c, jnp.asarray(overlap))
    blk = jnp.arange(ns)[None, :]
    cur = (pos // SEL_BLOCK)[:, None]
    forced = (blk == 0) | (blk == cur) | (blk == cur - 1)
    imp = jnp.where(forced, BIG, jnp.where(blk <= cur, imp, -BIG))
    _, idx = lax.top_k(imp, n_top)

    ksb = k_slc.reshape(B, ns, SEL_BLOCK, G, dh).transpose(0, 3, 1, 2, 4)
    vsb = v_slc.reshape(B, ns, SEL_BLOCK, G, dh).transpose(0, 3, 1, 2, 4)
    nq = T // Q_BLOCK
    qch = qg.reshape(B, nq, Q_BLOCK, G, hpg, dh).transpose(1, 0, 3, 4, 2, 5)
    ich = idx.reshape(B, G, nq, Q_BLOCK, n_top).transpose(2, 0, 1, 3, 4)
    gather = jax.vmap(jax.vmap(lambda blocks, ids: blocks[ids]))
    tok = jnp.arange(SEL_BLOCK)
    bias_g = bias_heads.reshape(G, hpg, REL_BUCKETS)
    group_bias = jax.vmap(lambda tab, bk: jnp.moveaxis(tab[:, bk], 0, 1), in_axes=(0, 1), out_axes=1)
    nk = n_top * SEL_BLOCK

    def sel_block(args):
        qc, ic, n = args
        kg = gather(ksb, ic).reshape(B, G, Q_BLOCK, nk, dh)
        vg = gather(vsb, ic).reshape(B, G, Q_BLOCK, nk, dh)
        kpos = (ic[..., None] * SEL_BLOCK + tok).reshape(B, G, Q_BLOCK, nk)
        qpos = n * Q_BLOCK + jnp.arange(Q_BLOCK)
        dist = qpos[:, None] - kpos
        l = jnp.einsum('bghqd,bgqkd->bghqk', qc, kg).astype(jnp.float32) * scale
        l = l + group_bias(bias_g, t5_bucket(dist))
        p = jax.nn.softmax(jnp.where((dist >= 0)[:, :, None], l, NEG), axis=-1)
        return jnp.einsum('bghqk,bgqkd->bqghd', p.astype(vg.dtype), vg)

    o_slc = lax.map(sel_block, (qch, ich, jnp.arange(nq)))
    o_slc = o_slc.transpose(1, 0, 2, 3, 4, 5).reshape(B, T, H, dh)

    o_win = banded_attention(q, k_win, v_win, NSA_WINDOW, bias_heads, None)

    g = jax.nn.sigmoid(gates.astype(jnp.float32)).astype(q.dtype)
    o = (g[..., 0:1] * o_cmp.reshape(B, T, H, dh) + g[..., 1:2] * o_slc + g[..., 2:3] * o_win)
    return o.reshape(B, T, H * dh)


def hybrid_layer(x, w_in, w_out, g_pre, g_post, cmp_pos, cmp_w1, cmp_w2,
                 q_norm, w_uq, kv_norm, w_ukv, sinks, rel_bias):
    B, T, _ = x.shape
    pos = jnp.arange(T)
    h = rms_norm(x, g_pre)
    a_q, a_kv, a_g, b_cq, b_ckv, b_kr, c_q, c_kv, z = split_cols(h @ w_in)

    o_a = nsa_attention(a_q.reshape(B, T, NSA_HEADS, HEAD_DIM),
                        a_kv.reshape(B, T, 6, NSA_KV_HEADS, HEAD_DIM),
                        a_g.reshape(B, T, NSA_HEADS, 3),
                        cmp_pos, cmp_w1, cmp_w2, rel_bias[:NSA_HEADS])

    qb = (rms_norm(b_cq, q_norm) @ w_uq).reshape(B, T, MLA_HEADS, MLA_NOPE + MLA_ROPE)
    q_full = jnp.concatenate([qb[..., :MLA_NOPE], rope(qb[..., MLA_NOPE:], pos)], axis=-1)
    kvb = (rms_norm(b_ckv, kv_norm) @ w_ukv).reshape(B, T, MLA_HEADS, MLA_NOPE + MLA_V)
    k_rope = rope(b_kr.reshape(B, T, 1, MLA_ROPE), pos)
    k_full = jnp.concatenate([kvb[..., :MLA_NOPE],
                              jnp.broadcast_to(k_rope, (B, T, MLA_HEADS, MLA_ROPE))], axis=-1)
    o_b = causal_block_attention(q_full, k_full, kvb[..., MLA_NOPE:], (MLA_NOPE + MLA_ROPE) ** -0.5)

    ckv = c_kv.reshape(B, T, 2, SWA_KV_HEADS, HEAD_DIM)
    o_c = banded_attention(c_q.reshape(B, T, SWA_HEADS, HEAD_DIM), ckv[:, :, 0], ckv[:, :, 1],
                           SWA_WINDOW, rel_bias[NSA_HEADS:], sinks)

    mixed = jnp.concatenate([o_a, o_b.reshape(B, T, W_B), o_c.reshape(B, T, W_C)], axis=-1) * jax.nn.silu(z)
    return x + rms_norm(mixed @ w_out, g_post)


def setup_inputs(seed: int = 0) -> dict:
    key = jax.random.key(seed)
    ks = jax.random.split(key, 14)
    f32 = jnp.float32

    def nrm(k, shape, scale):
        return jax.random.normal(k, shape, f32) * scale

    return {
        'x': nrm(ks[0], (BATCH, SEQ, D_MODEL), 1.0),
        'w_in': nrm(ks[1], (DEPTH, D_MODEL, D_IN), D_MODEL ** -0.5),
        'w_out': nrm(ks[2], (DEPTH, D_MIX, D_MODEL), D_MIX ** -0.5),
        'norm_pre': 1.0 + nrm(ks[3], (DEPTH, D_MODEL), 0.05),
        'norm_post': 1.0 + nrm(ks[4], (DEPTH, D_MODEL), 0.05),
        'cmp_pos': nrm(ks[5], (DEPTH, 2, CMP_LEN, HEAD_DIM), 0.1),
        'cmp_w1': nrm(ks[6], (DEPTH, 2, CMP_LEN * HEAD_DIM, CMP_HIDDEN), (CMP_LEN * HEAD_DIM) ** -0.5),
        'cmp_w2': nrm(ks[7], (DEPTH, 2, CMP_HIDDEN, HEAD_DIM), CMP_HIDDEN ** -0.5),
        'mla_q_norm': 1.0 + nrm(ks[8], (DEPTH, MLA_Q_RANK), 0.05),
        'mla_w_uq': nrm(ks[9], (DEPTH, MLA_Q_RANK, MLA_HEADS * (MLA_NOPE + MLA_ROPE)), MLA_Q_RANK ** -0.5),
        'mla_kv_norm': 1.0 + nrm(ks[10], (DEPTH, MLA_KV_RANK), 0.05),
        'mla_w_ukv': nrm(ks[11], (DEPTH, MLA_KV_RANK, MLA_HEADS * (MLA_NOPE + MLA_V)), MLA_KV_RANK ** -0.5),
        'swa_sinks': nrm(ks[12], (DEPTH, SWA_HEADS), 0.5),
        'rel_bias': nrm(ks[13], (N_BIAS_HEADS, REL_BUCKETS), 0.5),
    }


def reference(x, w_in, w_out, norm_pre, norm_post, cmp_pos, cmp_w1, cmp_w2,
              mla_q_norm, mla_w_uq, mla_kv_norm, mla_w_ukv, swa_sinks, rel_bias):
    for l in range(DEPTH):
        x = hybrid_layer(x, w_in[l], w_out[l], norm_pre[l], norm_post[l],
                         cmp_pos[l], cmp_w1[l], cmp_w2[l],
                         mla_q_norm[l], mla_w_uq[l], mla_kv_norm[l], mla_w_ukv[l],
                         swa_sinks[l], rel_bias)
    return x
```

```python
import contextlib
import math
import numpy as np
import ml_dtypes
import concourse.bass as bass
import concourse.mybir as mybir
from concourse.bass_utils import run_bass_kernel_spmd

F32 = mybir.dt.float32
BF16 = mybir.dt.bfloat16
AF = mybir.ActivationFunctionType
ALU = mybir.AluOpType

T = 4096
D = 1024
DIN = 2732
NQB = 32
NG = 8
NEGM = -30000.0
BIG = 1e9
C_AQ, C_KC, C_VC, C_KS, C_VS, C_KW, C_VW, C_G, C_CQ, C_CKV, C_KR, C_CQ2, C_CK, C_CV, C_Z = (
    0, 256, 320, 384, 448, 512, 576, 640, 652, 908, 1036, 1068, 1580, 1644, 1708)
HEADPOS = [0, 2, 1, 3]


class _Op:
    __slots__ = ("eng", "fn", "deps", "is_dma", "dma_sem", "dma_val", "flag", "count", "n_dma")

    def __init__(self, eng, fn, is_dma=False):
        self.eng = eng
        self.fn = fn
        self.deps = []
        self.is_dma = is_dma
        self.dma_sem = None
        self.dma_val = 0
        self.flag = False
        self.count = 0
        self.n_dma = 1


class Sched:
    ENGS = ("pe", "act", "dve", "pool", "sp")

    def __init__(self, nc):
        self.nc = nc
        self.ops = {e: [] for e in self.ENGS}
        self.bufs = {}
        self.dma_keys = {}
        self.n_dma_sems = 0
        self.stopped = False

    def _dep(self, op, other, raw=False):
        if other is None or other is op:
            return
        if other.eng == op.eng and not other.is_dma and not op.is_dma:
            if op.eng == "pe" or (not raw and op.eng != "pool"):
                return
        op.deps.append(other)
        if not other.is_dma:
            other.flag = True

    def op(self, eng, fn, reads=(), writes=()):
        if self.stopped:
            return None
        o = _Op(eng, fn)
        self._track(o, reads, writes)
        self.ops[eng].append(o)
        return o

    def dma(self, eng, fn, semkey, reads=(), writes=(), n=1):
        if self.stopped:
            return None
        o = _Op(eng, fn, is_dma=True)
        o.n_dma = n
        if semkey not in self.dma_keys:
            self.dma_keys[semkey] = [self.n_dma_sems, 0]
            self.n_dma_sems += 1
        st = self.dma_keys[semkey]
        st[1] += 16 * n
        o.dma_sem = st[0]
        o.dma_val = st[1]
        self._track(o, reads, writes)
        self.ops[eng].append(o)
        return o

    def _track(self, o, reads, writes):
        for k in reads:
            st = self.bufs.setdefault(k, [None, []])
            self._dep(o, st[0], raw=True)
        for k in writes:
            st = self.bufs.setdefault(k, [None, []])
            self._dep(o, st[0])
            for r in st[1]:
                self._dep(o, r)
        for k in reads:
            self.bufs[k][1].append(o)
        for k in writes:
            st = self.bufs[k]
            st[0] = o
            st[1] = []

    def emit(self, final_waits=()):
        nc = self.nc
        for e in self.ENGS:
            c = 0
            for o in self.ops[e]:
                if o.flag and not o.is_dma:
                    c += 1
                    o.count = c
        with contextlib.ExitStack() as st:
            esem = {e: st.enter_context(nc.semaphore("s_" + e)) for e in self.ENGS}
            dsem = [st.enter_context(nc.semaphore("d%d" % i)) for i in range(self.n_dma_sems)]
            block = st.enter_context(nc.Block())
            stats = {}

            def body(e, engine):
                waited = {}
                nw = 0
                for o in self.ops[e]:
                    for d in o.deps:
                        if d.is_dma:
                            key = ("d", d.dma_sem)
                            sem = dsem[d.dma_sem]
                            val = d.dma_val
                        else:
                            key = ("e", d.eng)
                            sem = esem[d.eng]
                            val = d.count
                        if waited.get(key, -1) >= val:
                            continue
                        waited[key] = val
                        engine.wait_ge(sem, val)
                        nw += 1
                    r = o.fn(engine)
                    if o.is_dma:
                        assert len(r) == o.n_dma, (len(r), o.n_dma)
                        for ins in r:
                            ins.then_inc(dsem[o.dma_sem], 16)
                    elif o.flag:
                        r.then_inc(esem[e], 1)
                if e == "sp":
                    for d in final_waits:
                        engine.wait_ge(dsem[d.dma_sem], d.dma_val)
                stats[e] = (len(self.ops[e]), nw)

            @block.tensor
            def _(eng):
                body("pe", eng)

            @block.scalar
            def _(eng):
                body("act", eng)

            @block.vector
            def _(eng):
                body("dve", eng)

            @block.gpsimd
            def _(eng):
                body("pool", eng)

            @block.sync
            def _(eng):
                body("sp", eng)
        self.stats = stats


def _t5_bucket_np(dist):
    dist = np.maximum(dist, 0)
    exact = 16
    large = exact + (np.log(np.maximum(dist, 1).astype(np.float32) / exact)
                     / math.log(512 / exact) * (32 - exact)).astype(np.int32)
    large = np.minimum(large, 31)
    return np.where(dist < exact, dist, large)


_CONST_CACHE = {}


def _const_tables():
    if _CONST_CACHE:
        return _CONST_CACHE
    bucket = _t5_bucket_np
    c = {}
    kk = np.arange(128)[:, None]
    qq = np.arange(128)[None, :]
    tokb = []
    for dl in range(6):
        dist = dl * 128 + qq - kk
        if dl == 5:
            dist = np.full_like(dist, 1000)
        tokb.append((bucket(dist), dist))
    c["tok"] = tokb
    cm = []
    for m in range(21):
        dist = 128 * m + qq - 16 * kk - 31
        if m == 20:
            dist = np.full_like(dist, 100000)
        cm.append((bucket(dist), dist))
    c["cmp"] = cm
    half = 16
    inv = (10000.0 ** (-np.arange(half, dtype=np.float32) / half)).astype(np.float32)
    ang = np.arange(T, dtype=np.float32)[None, :] * inv[:, None]
    cs = np.zeros((128, 2, T), np.float32)
    cs[64:80, 0] = np.cos(ang)
    cs[80:96, 0] = np.cos(ang)
    cs[64:80, 1] = np.sin(ang)
    cs[80:96, 1] = np.sin(ang)
    c["ropecs"] = cs
    c["ident"] = np.eye(128, dtype=np.float32)
    se = np.zeros((128, 128), np.float32)
    se[64, :] = 1.0
    so = np.zeros((128, 128), np.float32)
    so[0, :] = 1.0
    c["selsum"] = np.stack([se, so], axis=1)
    gs = np.zeros((128, 12, 128), np.float32)
    for r in range(12):
        gs[r, r, :] = 1.0
    c["gsel"] = gs
    ex = np.zeros((128, T), np.float32)
    for s in range(64):
        ex[s, s * 64:(s + 1) * 64] = 1.0
    c["expand"] = ex
    cstart = np.arange(256) * 16
    sstart = np.arange(64) * 64
    ov = (np.clip(np.minimum(cstart[:, None] + 32, sstart[None, :] + 64)
                  - np.maximum(cstart[:, None], sstart[None, :]), 0, None) / 16).astype(np.float32)
    ov[255, :] = 0.0
    ov1 = np.zeros((128, 2, 65), np.float32)
    for j in range(2):
        ov1[:, j, :64] = ov[j * 128:(j + 1) * 128]
        ov1[:, j, 64] = 1.0
    c["ovl1"] = ov1
    aw = np.zeros((128, 128), np.float32)
    bw = np.zeros((128, 128), np.float32)
    for q in range(128):
        for u in range(128):
            rel = (u - 63) - (1 if q >= 64 else 0)
            if rel <= -2:
                aw[q, u] = 1.0
            elif rel <= 0:
                bw[q, u] = BIG
            else:
                bw[q, u] = -BIG
    c["awbw"] = np.stack([aw, bw], axis=1)
    dm = np.where(qq - kk >= 0, 0.0, NEGM).astype(np.float32)
    c["mlamask"] = dm
    _CONST_CACHE.update(c)
    return c


def _bias_tiles(rel_bias):
    c = _const_tables()
    rb = np.asarray(rel_bias, np.float32)

    def tile4(heads, bk, valid):
        out = np.empty((128, 512), np.float32)
        for pi in range(4):
            h = heads[HEADPOS[pi]]
            g = rb[h][bk]
            out[:, pi * 128:(pi + 1) * 128] = np.where(valid, g, np.float32(NEGM))
        return out

    a_heads = [0, 1, 2, 3]
    slcb = np.empty((128, 6, 512), np.float32)
    for dl in range(6):
        bk, dist = c["tok"][dl]
        slcb[:, dl] = tile4(a_heads, bk, dist >= 0)
    bk, dist = c["tok"][4]
    winb4 = tile4(a_heads, bk, (dist >= 0) & (dist < 512))
    swab = np.empty((128, 2, 2, 512), np.float32)
    for g in range(2):
        heads = [4 + 4 * g + i for i in range(4)]
        for dl in range(2):
            bk, dist = c["tok"][dl]
            swab[:, g, dl] = tile4(heads, bk, (dist >= 0) & (dist < 128))
    cmpb = np.empty((21, 128, 512), np.float32)
    for m in range(21):
        bk, dist = c["cmp"][m]
        valid = dist >= 0
        if m == 20:
            valid = np.ones_like(valid)
        cmpb[m] = tile4(a_heads, bk, valid)
    return slcb, winb4, swab, cmpb


class _Stop(Exception):
    pass


def build(n_layers=4, debug=False, stop_after=None, pcut=None):
    nc = bass.Bass("TRN2", target_bir_lowering=False)
    L = n_layers
    S = Sched(nc)

    def din(name, shape, dt=F32):
        return nc.dram_tensor(name, list(shape), dt, kind="ExternalInput").ap()

    dbgset = set(debug) if debug else set()

    def dscr(name, shape, dt):
        return nc.dram_tensor(name, list(shape), dt, kind=("ExternalOutput" if name in dbgset else "Internal")).ap()

    xT_d = din("xT", [8, 128, T])
    w_in_d = din("w_in", [L, 8, 128, DIN])
    w_out_d = din("w_out", [L, 8, 128, D])
    gpre_d = din("gpre", [L, 128, 8])
    gpost_d = din("gpost", [L, 128, 8])
    peT_d = din("peT", [L, 128, 32])
    w1_d = din("w1", [L, 2, 32, 64, 128])
    w2_d = din("w2", [L, 2, 128, 64])
    qn_d = din("qn", [L, 128, 2])
    wuq_d = din("wuq", [L, 2, 128, 384])
    kvn_d = din("kvn", [L, 128, 1])
    wukv_d = din("wukv", [L, 128, 512])
    sinks_d = din("sinks", [L, 128, 8])
    slcb_d = din("slcb", [128, 6, 512])
    winb4_d = din("winb4", [128, 512])
    swab_d = din("swab", [128, 2, 2, 512])
    cmpb_d = din("cmpb", [21, 128, 512])
    ident_d = din("ident", [128, 128])
    selsum_d = din("selsum", [128, 2, 128])
    gsel_d = din("gsel", [128, 12, 128])
    expand_d = din("expand", [128, T])
    ovl1_d = din("ovl1", [128, 2, 65])
    awbw_d = din("awbw", [128, 2, 128])
    mlamask_d = din("mlamask", [128, 128])
    ropecs_d = din("ropecs", [128, 2, T])
    y_d = nc.dram_tensor("y", [8, 128, T], F32, kind="ExternalOutput").ap()

    qaT_s = dscr("qaT_s", [64, NQB, 512], BF16)
    kvcT_s = dscr("kvcT_s", [128, T], BF16)
    ksT_s = dscr("ksT_s", [64, T], BF16)
    kwT_s = dscr("kwT_s", [64, T], BF16)
    vsA_s = dscr("vsA_s", [T, 192], BF16)
    vwA_s = dscr("vwA_s", [T, 192], BF16)
    vcA_s = dscr("vcA_s", [T, 192], BF16)
    sg_s = dscr("sg_s", [12, T], F32)
    qbT_s = dscr("qbT_s", [4, 96, T], BF16)
    kbT_s = dscr("kbT_s", [4, 96, T], BF16)
    vbA_s = dscr("vbA_s", [4, T, 128], BF16)
    qcT_s = dscr("qcT_s", [2, 64, NQB, 512], BF16)
    kcT_s = dscr("kcT_s", [64, T], BF16)
    szT_s = dscr("szT_s", [8, 128, T], BF16)
    oT_s = dscr("oT_s", [8, 128, T], F32)
    cmpbb_s = dscr("cmpbb_s", [21, 128, 512], BF16)
    expand_s = dscr("expand_s", [64, T], BF16)

    with contextlib.ExitStack() as st:
        def sb(name, shape, dt):
            return st.enter_context(nc.sbuf_tensor("sb_" + name, list(shape), dt))

        arena = sb("arena", [128, 32768], BF16)
        f32a = sb("f32a", [128, 8, 512], F32)
        f32b = sb("f32b", [128, 8, 512], F32)
        bf8s = [sb("bf8_%d" % i, [128, 8, 512], BF16) for i in range(2)]
        sqb = [sb("sqb%d" % i, [128, 512], BF16) for i in range(2)]
        rstd_t = sb("rstd_t", [128, 512], F32)
        wsm = sb("wsm", [128, 768], F32)
        wuq_bf = sb("wuq_bf", [128, 2, 384], BF16)
        wuqrot = sb("wuqrot", [128, 2, 384], BF16)
        wukv_bf = sb("wukv_bf", [128, 512], BF16)
        wkrrot = sb("wkrrot", [128, 8, 96], BF16)
        wkr96 = sb("wkr96", [128, 8, 96], BF16)
        w2bf = sb("w2bf", [128, 2, 64], BF16)
        pebf = sb("pebf", [128, 32], BF16)
        hc = sb("hc", [128, 2], F32)
        gpre = sb("gpre", [128, 8], F32)
        gpost = sb("gpost", [128, 8], F32)
        qn = sb("qn", [128, 2], F32)
        kvn = sb("kvn", [128, 1], F32)
        sinks = sb("sinks", [128, 8], F32)
        esb = sb("esb", [128, 2, 512], F32)
        slcb = sb("slcb", [128, 6, 512], BF16)
        winb4 = sb("winb4", [128, 512], BF16)
        swab = sb("swab", [128, 2, 2, 512], BF16)
        ident_bf = sb("ident_bf", [128, 128], BF16)
        ident_f = sb("ident_f", [128, 128], F32)
        ones_bf = sb("ones_bf", [128, 128], BF16)
        mlamask = sb("mlamask", [128, 128], BF16)
        selsum = sb("selsum", [128, 2, 128], F32)
        gsel = sb("gsel", [128, 12, 128], F32)
        ovl1 = sb("ovl1", [128, 2, 65], BF16)
        awbw = sb("awbw", [128, 2, 128], F32)
        cs = sb("cs", [128, 2, 512], F32)
        stg = [sb("stg%d" % i, [128, 512], BF16) for i in range(3)]
        stf = [sb("stf%d" % i, [128, 512], F32) for i in range(3)]
        cqn = sb("cqn", [128, 2, 512], BF16)
        ckvn = sb("ckvn", [128, 512], BF16)
        vbst = sb("vbst", [128, 4, 4, 128], BF16)
        vast = [sb("vast%d" % i, [128, 4, 192], BF16) for i in range(3)]
        sT = [sb("sT%d" % i, [128, 256], BF16) for i in range(2)]
        kcT = sb("kcT", [128, 256], BF16)
        vcA = sb("vcA", [128, 2, 192], BF16)
        ebuf = [sb("ebuf%d" % i, [128, 512], BF16) for i in range(3)]
        qbuf = [sb("qbuf%d" % i, [128, 512], BF16) for i in range(3)]
        cbt = [sb("cbt%d" % i, [128, 512], BF16) for i in range(2)]
        sgb = [sb("sgb%d" % i, [128, 128], F32) for i in range(2)]
        nmq = [sb("nmq%d" % i, [128, 512], BF16) for i in range(2)]
        oacc = [sb("oacc%d" % i, [128, 512], F32) for i in range(2)]
        xe = [sb("xe%d" % i, [128, 512], F32) for i in range(2)]
        rr = [sb("rr%d" % i, [128, 512], F32) for i in range(2)]
        ost = [sb("ost%d" % i, [128, 512], F32) for i in range(2)]
        rs4 = sb("rs4", [128, 4], F32)
        eps_t = sb("eps_t", [128, 2], F32)
        impS = sb("impS", [128, 64], F32)
        imp2 = sb("imp2", [128, 64], F32)
        imp3 = sb("imp3", [128, 64], F32)
        m8 = sb("m8", [128, 16], F32)
        nmts = [sb("nmt%d" % i, [128, 128], F32) for i in range(2)]
        ps = [st.enter_context(nc.psum_tensor("ps%d" % i, [128, 512], F32)) for i in range(8)]
        PS = ["ps%d" % i for i in range(8)]

        cqf = f32b[:, 0:2, :]
        win_bf = arena[:, 0:8 * DIN].rearrange("p (k c) -> p k c", k=8)
        WIN = ["win%d" % k for k in range(8)]
        AR = ["ar%d" % i for i in range(8)]

        def slot(i, n=1):
            return arena[:, i * 4096:(i + n) * 4096]
        w1bf = slot(6).rearrange("p (j m) -> p j m", j=32)
        wout_bf = slot(0, 2).rearrange("p (k c) -> p k c", k=8)

        def mm(out, lhsT, rhs, start, stop, r, w):
            S.op("pe", lambda e: e.matmul(out, lhsT=lhsT, rhs=rhs, start=start, stop=stop), r, w)

        def act(out, in_, func, r, w, bias=None, scale=None):
            kw = {}
            if bias is not None:
                kw["bias"] = bias
            if scale is not None:
                kw["scale"] = scale
            S.op("act", lambda e: e.activation(out=out, in_=in_, func=func, **kw), r, w)

        def tt(eng, out, in0, in1, op, r, w):
            S.op(eng, lambda e: e.tensor_tensor(out=out, in0=in0, in1=in1, op=op), r, w)

        def ts(eng, out, in0, s1, s2, op0, op1, r, w):
            if op1 is None:
                S.op(eng, lambda e: e.tensor_scalar(out=out, in0=in0, scalar1=s1, scalar2=None, op0=op0), r, w)
            else:
                S.op(eng, lambda e: e.tensor_scalar(out=out, in0=in0, scalar1=s1, scalar2=s2, op0=op0, op1=op1), r, w)

        def stt(out, in0, scalar, in1, op0, op1, r, w):
            S.op("dve", lambda e: e.scalar_tensor_tensor(out=out, in0=in0, scalar=scalar, in1=in1, op0=op0, op1=op1), r, w)

        def cp(eng, out, in_, r, w):
            if eng == "act":
                act(out, in_, AF.Identity, r, w)
            else:
                S.op(eng, lambda e: e.tensor_copy(out=out, in_=in_), r, w)

        def recip(out, in_, r, w):
            S.op("dve", lambda e: e.reciprocal(out=out, in_=in_), r, w)

        def memset(eng, ap, val, w):
            S.op(eng, lambda e: e.memset(ap, val), (), w)

        def dma(eng, out, in_, semkey, r, w):
            return S.dma(eng, lambda e: [e.dma_start(out=out, in_=in_)], semkey, r, w)

        rot = {"evac": 0, "stg": 0, "stf": 0, "ps": 0}

        def evac(out, in_, scale, r, w):
            rot["evac"] += 1
            if rot["evac"] % 2 == 0:
                act(out, in_, AF.Identity, r, w, scale=scale)
            else:
                ts("dve", out, in_, 1.0 if scale is None else scale, None, ALU.mult, None, r, w)

        def next_stg():
            rot["stg"] = (rot["stg"] + 1) % 3
            return stg[rot["stg"]], "stg%d" % rot["stg"]

        def next_stf():
            rot["stf"] = (rot["stf"] + 1) % 3
            return stf[rot["stf"]], "stf%d" % rot["stf"]

        def next_ps(lo=0, hi=8):
            rot["ps"] = rot["ps"] + 1
            i = lo + rot["ps"] % (hi - lo)
            return ps[i], PS[i]

        def load_const(dst, src, cols, key, cast_dst=None, rows=128):
            pass

        dma("sp", ident_f[:], ident_d[:, :], "c_ident_f", (), ["ident_f"])
        dma("sp", selsum[:], selsum_d[:, :, :], "c_selsum", (), ["selsum"])
        dma("sp", gsel[:], gsel_d[:, :, :], "c_gsel", (), ["gsel"])
        dma("sp", awbw[:], awbw_d[:, :, :], "c_awbw", (), ["awbw"])
        memset("pool", ones_bf[:], 1.0, ["ones_bf"])
        memset("pool", eps_t[:, 0:1], 1e-6, ["eps_t"])
        memset("pool", eps_t[:, 1:2], 1e-30, ["eps_t"])
        cp("dve", ident_bf[:], ident_f[:], ["ident_f"], ["ident_bf"])
        f32a_flat = f32a[:].rearrange("p k c -> p (k c)")
        f32b_flat = f32b[:].rearrange("p k c -> p (k c)")
        dma("sp", f32a_flat[:, 0:3072], slcb_d.rearrange("p a c -> p (a c)"), "f32a", (), ["f32a"])
        dma("sp", f32b_flat[:, 0:512], winb4_d[:, :], "f32b", (), ["f32b"])
        tt("dve", f32b_flat[:, 0:512], f32b_flat[:, 0:512], f32a_flat[:, 2560:3072], ALU.subtract, ["f32b", "f32a"], ["f32b"])
        for dl in range(5):
            tt("dve", f32a_flat[:, dl * 512:(dl + 1) * 512], f32a_flat[:, dl * 512:(dl + 1) * 512], f32a_flat[:, 2560:3072],
               ALU.subtract, ["f32a"], ["f32a"])
        cp("dve", slcb[:].rearrange("p a c -> p (a c)"), f32a_flat[:, 0:3072], ["f32a"], ["slcb"])
        dma("sp", f32b_flat[:, 512:2560], swab_d.rearrange("p a b c -> p (a b c)"), "f32b", (), ["f32b"])
        dma("sp", f32b_flat[:, 2560:2688], mlamask_d[:, :], "f32b", (), ["f32b"])
        dma("sp", f32b_flat[:, 2688:2818], ovl1_d.rearrange("p a c -> p (a c)"), "f32b", (), ["f32b"])
        cp("pool", winb4[:], f32b_flat[:, 0:512], ["f32b"], ["winb4"])
        cp("pool", swab[:].rearrange("p a b c -> p (a b c)"), f32b_flat[:, 512:2560], ["f32b"], ["swab"])
        cp("pool", mlamask[:], f32b_flat[:, 2560:2688], ["f32b"], ["mlamask"])
        cp("pool", ovl1[:].rearrange("p a c -> p (a c)"), f32b_flat[:, 2688:2818], ["f32b"], ["ovl1"])
        dma("sp", f32a_flat[:, 0:4096], expand_d[:, :], "f32a", (), ["f32a"])
        cp("dve", slot(7)[0:64, :], f32a_flat[0:64, 0:4096], ["f32a"], [AR[7]])
        dma("pool", expand_s[:, :], slot(7)[0:64, :], "ar7_st", [AR[7]], ["expand_s"])
        for i in range(2):
            memset("pool", nmts[i][:], 0.0, ["nmt%d" % i])
        for i in range(3):
            memset("pool", qbuf[i][:], 0.0, ["qbuf%d" % i, "qbuf%dn" % i])
        for i in range(2):
            memset("pool", sgb[i][:], 0.0, ["sgb%d" % i])
        dma("sp", ost[0][:], cmpb_d[20, :, :], "ost0", (), ["ost0c"])
        for m in range(20):
            sf, sfk = next_stf()
            sg_, sgk = next_stg()
            dma("sp", sf[:], cmpb_d[m, :, :], sfk, (), [sfk])
            tt("pool" if m % 2 else "dve", sg_[:], sf[:], ost[0][:], ALU.subtract, [sfk, "ost0c"], [sgk])
            dma("pool", cmpbb_s[m, :, :], sg_[:], sgk + "_st", [sgk], ["cmpbb"])
        memset("pool", vbst[:], 0.0, ["vbst"])
        for h in range(4):
            col = 64
            memset("pool", vbst[:, :, h, col:col + 1], 1.0, ["vbst"])
        for i in range(3):
            memset("pool", vast[i][:], 0.0, ["vast%d" % i])
            memset("pool", vast[i][:, :, 64:65], 1.0, ["vast%d" % i])
        memset("pool", vcA[:], 0.0, ["vcA"])
        memset("pool", vcA[:, :, 64:65], 1.0, ["vcA"])
        memset("pool", sT[0][:], 0.0, ["sT0"])
        memset("pool", sT[1][:], 0.0, ["sT1"])
        memset("pool", kcT[:], 0.0, ["kcT"])
        memset("pool", wuqrot[:], 0.0, ["wuq"])
        memset("pool", wkrrot[:], 0.0, ["wkrrot"])
        memset("pool", wkr96[:], 0.0, ["wkrrot"])

        S._final = []

        def chk(i):
            if pcut is not None and i == pcut:
                S.stopped = True
        SC_B = 96.0 ** -0.5
        for l in range(L):
            xsrc = xT_d if l == 0 else y_d
            last_layer = (l == L - 1)
            dma("sp", gpre[:], gpre_d[l, :, :], "gpre", (), ["gpre"])
            dma("sp", gpost[:], gpost_d[l, :, :], "gpost", (), ["gpost"])
            dma("sp", qn[:], qn_d[l, :, :], "qn", (), ["qn"])
            dma("sp", kvn[:], kvn_d[l, :, :], "kvn", (), ["kvn"])
            dma("sp", sinks[:], sinks_d[l, :, :], "sinks", (), ["sinks"])
            for k in range(8):
                fl, fk = (f32a_flat, "f32a") if k % 2 == 0 else (f32b_flat, "f32b")
                dma("sp", fl[:, 0:DIN], w_in_d[l, k, :, :], fk, (), [fk])
                ts("dve" if k % 2 == 0 else "pool", win_bf[:, k, :], fl[:, 0:DIN], gpre[:, k:k + 1], None, ALU.mult, None,
                   [fk, "gpre"], [WIN[k], "wout"] + AR)
                ts("dve", wkrrot[:, k, 64:80], fl[:, C_KR + 16:C_KR + 32], gpre[:, k:k + 1], -1.0, ALU.mult, ALU.mult, [fk, "gpre"], ["wkrrot"])
                ts("dve", wkrrot[:, k, 80:96], fl[:, C_KR:C_KR + 16], gpre[:, k:k + 1], None, ALU.mult, None, [fk, "gpre"], ["wkrrot"])
                ts("dve", wkr96[:, k, 64:96], fl[:, C_KR:C_KR + 32], gpre[:, k:k + 1], None, ALU.mult, None, [fk, "gpre"], ["wkrrot"])
            dma("sp", f32a_flat[0:64, 0:4096], w1_d[l, 0].rearrange("j d m -> d j m"), "f32a", (), ["f32a"])
            dma("sp", f32b_flat[64:128, 0:4096], w1_d[l, 1].rearrange("j d m -> d j m"), "f32b", (), ["f32b"])
            cp("dve", slot(6)[0:64, :], f32a_flat[0:64, 0:4096], ["f32a"], ["w1k", AR[6], AR[7]])
            cp("pool", slot(6)[64:128, :], f32b_flat[64:128, 0:4096], ["f32b"], ["w1v", AR[6], AR[7]])
            dma("sp", wsm[:, 0:768], wuq_d[l].rearrange("c p n -> p c n"), "wsm", (), ["wsm"])
            for c in range(2):
                ts("dve", wuq_bf[:, c, :], wsm[:, c * 384:(c + 1) * 384], qn[:, c:c + 1], None, ALU.mult, None, ["wsm", "qn"], ["wuq"])
                for h in range(4):
                    b0 = c * 384 + h * 96 + 64
                    ts("dve", wuqrot[:, c, h * 96 + 64:h * 96 + 80], wsm[:, b0 + 16:b0 + 32], qn[:, c:c + 1], -1.0, ALU.mult, ALU.mult, ["wsm", "qn"], ["wuq"])
                    ts("dve", wuqrot[:, c, h * 96 + 80:h * 96 + 96], wsm[:, b0:b0 + 16], qn[:, c:c + 1], None, ALU.mult, None, ["wsm", "qn"], ["wuq"])
            dma("sp", wsm[:, 0:512], wukv_d[l, :, :], "wsm", (), ["wsm"])
            ts("dve", wukv_bf[:], wsm[:, 0:512], kvn[:, 0:1], None, ALU.mult, None, ["wsm", "kvn"], ["wukv"])
            dma("sp", wsm[:, 0:128].rearrange("p (i d) -> p i d", i=2), w2_d[l].rearrange("i m d -> m i d"), "wsm", (), ["wsm"])
            cp("dve", w2bf[:].rearrange("p i d -> p (i d)"), wsm[:, 0:128], ["wsm"], ["w2bf"])
            dma("sp", wsm[:, 0:32], peT_d[l, :, :], "wsm", (), ["wsm"])
            cp("dve", pebf[:], wsm[:, 0:32], ["wsm"], ["pebf"])
            for g in range(2):
                for pi in range(4):
                    hh = 4 * g + HEADPOS[pi]
                    act(esb[:, g, pi * 128:(pi + 1) * 128], ident_f[:], AF.Exp, ["ident_f", "sinks"], ["esb"],
                        bias=sinks[:, hh:hh + 1], scale=0.0)

            if stop_after == "W":
                break
            for G in range(NG if stop_after != "P1" else 1):
                tsl = slice(G * 512, (G + 1) * 512)
                YK = "y%d" % G
                hb, hk = bf8s[G % 2], "hT%d_" % (G % 2)
                dma("sp", f32a[:], xsrc.rearrange("k p t -> p k t")[:, :, tsl], "f32a", [YK], ["f32a"])
                dma("sp", cs[:], ropecs_d[:, :, tsl], "cs", (), ["cs"])
                ssb, ssk = ps[7], PS[7]
                for k in range(8):
                    q_, qk = sqb[k % 2], "sqb%d" % (k % 2)
                    if k % 2 == 0:
                        act(q_[:], f32a[:, k, :], AF.Square, ["f32a"], [qk])
                    else:
                        tt("pool", q_[:], f32a[:, k, :], f32a[:, k, :], ALU.mult, ["f32a"], [qk])
                    mm(ssb[:], ones_bf[:], q_[:], k == 0, k == 7, ["ones_bf", qk], [ssk])
                act(rstd_t[:], ssb[:], AF.Ln, [ssk], ["rstd"], bias=eps_t[:, 0:1], scale=1.0 / D)
                act(rstd_t[:], rstd_t[:], AF.Exp, ["rstd"], ["rstd"], scale=-0.5)
                for k in range(8):
                    tt("dve" if k % 2 == 0 else "pool", hb[:, k, :], f32a[:, k, :], rstd_t[:], ALU.mult, ["f32a", "rstd"], [hk + str(k)])

                def proj(col, M, pst, psk, prow=0):
                    for k in range(8):
                        mm(pst[prow:prow + M, :], win_bf[:, k, col:col + M], hb[:, k, :], k == 0, k == 7,
                           [WIN[k], hk + str(k)], [psk])

                chk(1)
                for (col0, dst, npair) in ((C_AQ, qaT_s, 2), (C_CQ2, None, 4)):
                    for p in range(npair):
                        pst, psk = next_ps(0, 6)
                        proj(col0 + p * 128, 128, pst, psk)
                        sg_, sgk = next_stg()
                        evac(sg_[:], pst[:], 0.125, [psk], [sgk])
                        if dst is not None:
                            tgt = dst
                            hp = p
                            key = "qaT%d" % G
                        else:
                            tgt = qcT_s[p // 2]
                            hp = p % 2
                            key = "qcT%d_%d" % (p // 2, G)
                        for par in range(2):
                            pi = par * 2 + hp
                            dma("pool", tgt[:, 4 * G:4 * G + 4, pi * 128:(pi + 1) * 128],
                                sg_[par * 64:(par + 1) * 64, :].rearrange("p (n q) -> p n q", n=4),
                                sgk + "_st", [sgk], [key])
                chk(2)
                pst, psk = next_ps(0, 6)
                proj(C_KC, 128, pst, psk)
                sg_, sgk = next_stg()
                evac(sg_[:], pst[:], None, [psk], [sgk])
                dma("pool", kvcT_s[:, tsl], sg_[:], sgk + "_st", [sgk], ["kvcT_s"])
                chk(3)
                for (col, dst, key) in ((C_KS, ksT_s, "ksT_s"), (C_KW, kwT_s, "kwT_s"), (C_CK, kcT_s, "kcT_s")):
                    pst, psk = next_ps(0, 6)
                    proj(col, 128, pst, psk)
                    sg_, sgk = next_stg()
                    evac(sg_[0:64, :], pst[0:64, :], None, [psk], [sgk])
                    dma("pool", dst[:, tsl], sg_[0:64, :], sgk + "_st", [sgk], [key])
                chk(4)
                pst, psk = next_ps(0, 6)
                proj(C_G, 128, pst, psk)
                sf, sfk = next_stf()
                act(sf[0:12, :], pst[0:12, :], AF.Sigmoid, [psk], [sfk])
                dma("pool", sg_s[:, tsl], sf[0:12, :], sfk + "_st", [sfk], ["sg_s"])
                chk(5)
                for c in range(2):
                    pst, psk = next_ps(0, 6)
                    proj(C_CQ + c * 128, 128, pst, psk)
                    cp("dve", xe[c][:], pst[:], [psk], ["xe%d" % c])
                    q_, qk = sqb[c % 2], "sqb%d" % (c % 2)
                    tt("pool", q_[:], xe[c][:], xe[c][:], ALU.mult, ["xe%d" % c], [qk])
                    mm(ssb[:], ones_bf[:], q_[:], c == 0, c == 1, ["ones_bf", qk], [ssk])
                act(rstd_t[:], ssb[:], AF.Ln, [ssk], ["rstd"], bias=eps_t[:, 0:1], scale=1.0 / 256)
                act(rstd_t[:], rstd_t[:], AF.Exp, ["rstd"], ["rstd"], scale=-0.5)
                for c in range(2):
                    tt("dve", cqn[:, c, :], xe[c][:], rstd_t[:], ALU.mult, ["xe%d" % c, "rstd"], ["cqn"])
                chk(50)
                for h in range(4):
                    pa, pak = next_ps(0, 6)
                    pb, pbk = next_ps(0, 6)
                    for c in range(2):
                        mm(pa[0:96, :], wuq_bf[:, c, h * 96:(h + 1) * 96], cqn[:, c, :], c == 0, c == 1, ["wuq", "cqn"], [pak])
                    for c in range(2):
                        mm(pb[0:96, :], wuqrot[:, c, h * 96:(h + 1) * 96], cqn[:, c, :], c == 0, c == 1, ["wuq", "cqn"], [pbk])
                    chk(51)
                    sg_, sgk = next_stg()
                    evac(sg_[0:64, :], pa[0:64, :], SC_B, [pak], [sgk + "a"])
                    chk(52)
                    f1, f1k = next_stf()
                    f2, f2k = next_stf()
                    stt(f1[64:96, :], pa[64:96, :], SC_B, cs[64:96, 0, :], ALU.mult, ALU.mult, [pak, "cs"], [f1k])
                    stt(f2[64:96, :], pb[64:96, :], SC_B, cs[64:96, 1, :], ALU.mult, ALU.mult, [pbk, "cs"], [f2k])
                    chk(53)
                    tt("dve", sg_[64:96, :], f1[64:96, :], f2[64:96, :], ALU.add, [f1k, f2k], [sgk + "b"])
                    chk(54)
                    dma("pool", qbT_s[h, :, tsl], sg_[0:96, :], sgk + "_st", [sgk + "a", sgk + "b"], ["qbT%d" % h])
                chk(6)
                pst, psk = next_ps(0, 6)
                proj(C_CKV, 128, pst, psk)
                cp("dve", xe[0][:], pst[:], [psk], ["xe0"])
                tt("pool", sqb[0][:], xe[0][:], xe[0][:], ALU.mult, ["xe0"], ["sqb0"])
                mm(ssb[:], ones_bf[:], sqb[0][:], True, True, ["ones_bf", "sqb0"], [ssk])
                act(rstd_t[:], ssb[:], AF.Ln, [ssk], ["rstd"], bias=eps_t[:, 0:1], scale=1.0 / 128)
                act(rstd_t[:], rstd_t[:], AF.Exp, ["rstd"], ["rstd"], scale=-0.5)
                tt("dve", ckvn[:], xe[0][:], rstd_t[:], ALU.mult, ["xe0", "rstd"], ["ckvn"])
                for h in range(4):
                    pst, psk = next_ps(0, 6)
                    mm(pst[:, :], wukv_bf[:, h * 128:(h + 1) * 128], ckvn[:], True, True, ["wukv", "ckvn"], [psk])
                    sg_, sgk = next_stg()
                    evac(sg_[0:64, :], pst[0:64, :], None, [psk], [sgk])
                    dma("pool", kbT_s[h, 0:64, tsl], sg_[0:64, :], sgk + "_st", [sgk], ["kbT%d" % h])
                wv = wukv_bf[:].rearrange("p (h c) -> p h c", h=4)
                for t4 in range(4):
                    pst, psk = next_ps(0, 6)
                    mm(pst[:, 0:256], ckvn[:, t4 * 128:(t4 + 1) * 128], wv[:, :, 64:128], True, True, ["wukv", "ckvn"], [psk])
                    for h in range(4):
                        c0 = 0
                        evac(vbst[:, t4, h, c0:c0 + 64], pst[:, h * 64:(h + 1) * 64], None, [psk], ["vbst"])
                for h in range(4):
                    dma("pool", vbA_s[h, tsl, :].rearrange("(t p) c -> p t c", p=128), vbst[:, :, h, :], "vbst_st", ["vbst"], ["vbA%d" % h])
                chk(7)
                pa, pak = next_ps(0, 6)
                pb, pbk = next_ps(0, 6)
                for k in range(8):
                    mm(pa[0:96, :], wkr96[:, k, :], hb[:, k, :], k == 0, k == 7, ["wkrrot", hk + str(k)], [pak])
                for k in range(8):
                    mm(pb[0:96, :], wkrrot[:, k, :], hb[:, k, :], k == 0, k == 7, ["wkrrot", hk + str(k)], [pbk])
                f1, f1k = next_stf()
                f2, f2k = next_stf()
                sg_, sgk = next_stg()
                tt("dve", f1[64:96, :], pa[64:96, :], cs[64:96, 0, :], ALU.mult, [pak, "cs"], [f1k])
                tt("dve", f2[64:96, :], pb[64:96, :], cs[64:96, 1, :], ALU.mult, [pbk, "cs"], [f2k])
                tt("dve", sg_[64:96, :], f1[64:96, :], f2[64:96, :], ALU.add, [f1k, f2k], [sgk])
                for h in range(4):
                    dma("pool", kbT_s[h, 64:96, tsl], sg_[64:96, :], sgk + "_st", [sgk], ["kbT%d" % h])
                chk(8)
                for c in range(8):
                    pst, psk = next_ps(0, 6)
                    proj(C_Z + c * 128, 128, pst, psk)
                    sg_, sgk = next_stg()
                    act(sg_[:], pst[:], AF.Silu, [psk], [sgk])
                    dma("pool", szT_s[c, :, tsl], sg_[:], sgk + "_st", [sgk], ["szT%d" % G])
                chk(9)
                for t4 in range(4):
                    for i, col in enumerate((C_VS, C_VW, C_CV)):
                        pst, psk = next_ps(0, 6)
                        for k in range(8):
                            mm(pst[:, 0:64], hb[:, k, t4 * 128:(t4 + 1) * 128], win_bf[:, k, col:col + 64],
                               k == 0, k == 7, [WIN[k], hk + str(k)], [psk])
                        evac(vast[i][:, t4, 0:64], pst[:, 0:64], None, [psk], ["vast%d" % i])
                        evac(vast[i][:, t4, 128:192], pst[:, 0:64], None, [psk], ["vast%d" % i])
                for i, (dst, key) in enumerate(((vsA_s, "vsA_s"), (vwA_s, "vwA_s"), (vcA_s, "vcA_s"))):
                    dma("pool", dst[tsl, :].rearrange("(t p) c -> p t c", p=128), vast[i][:], "vast%d_st" % i, ["vast%d" % i], [key])
            if stop_after == "P":
                break
            kvc = slot(0)
            dma("sp", kvc, kvcT_s[:, :], "ar0", ["kvcT_s"], [AR[0]] + WIN)
            for i in range(2):
                p0 = 64 * i
                wk = "w1k" if i == 0 else "w1v"
                ph, phk = next_ps(0, 6)
                for j in range(32):
                    mm(ph[:, 0:255], w1bf[p0:p0 + 64, j, :], kvc[p0:p0 + 64, j:j + 4065:16], j == 0, j == 31, [wk, AR[0]], [phk])
                pc, pck = next_ps(0, 6)
                for j in range(32):
                    mm(pc[:, 0:1], w1bf[p0:p0 + 64, j, :], pebf[p0:p0 + 64, j:j + 1], j == 0, j == 31, [wk, "pebf"], [pck])
                cp("dve", hc[:, i:i + 1], pc[:, 0:1], [pck], ["hc%d" % i])
                act(sT[i][:, 0:255], ph[:, 0:255], AF.Silu, [phk, "hc%d" % i], ["sT%d" % i], bias=hc[:, i:i + 1])
            pk, pkk = next_ps(0, 6)
            mm(pk[0:64, 0:256], w2bf[:, 0, :], sT[0][:], True, True, ["w2bf", "sT0"], [pkk])
            evac(kcT[0:64, :], pk[0:64, 0:256], None, [pkk], ["kcT"])
            for jt in range(2):
                pv, pvk = next_ps(0, 6)
                mm(pv[:, 0:64], sT[1][:, jt * 128:(jt + 1) * 128], w2bf[:, 1, :], True, True, ["w2bf", "sT1"], [pvk])
                evac(vcA[:, jt, 0:64], pv[:, 0:64], None, [pvk], ["vcA"])
                evac(vcA[:, jt, 128:192], pv[:, 0:64], None, [pvk], ["vcA"])
            if stop_after == "CMP":
                break

            def acc_bank():
                i = rot["acc"] = (rot.get("acc", 0) + 1) % 4
                return ps[2 + i], PS[2 + i]

            def next_e():
                ei = rot["e"] = (rot.get("e", 0) + 1) % 3
                return ebuf[ei], "ebuf%d" % ei

            def finalize(acc, acck, mode, n, br=None, first=False, last=False, g=0, ob=None, obk=None):
                i2 = rot["fin"] = (rot.get("fin", 0) + 1) % 2
                X_, xk = xe[i2], "xe%d" % i2
                R_, rk = rr[i2], "rr%d" % i2
                cp("act" if i2 == 0 else "dve", X_[0:65, :], acc[0:65, :], [acck], [xk])
                sbp, sbk = ps[6], PS[6]
                mm(sbp[:, :], selsum[0:65, 0, :], X_[0:65, :], True, True, ["selsum", xk], [sbk])
                if mode == "nsa":
                    gi = rot["gb"] = (rot.get("gb", 0) + 1) % 2
                    G_, gbk = ost[gi], "ost%d" % gi
                    for pi in range(4):
                        r_ = HEADPOS[pi] * 3 + br
                        S.dma("sp", (lambda G_=G_, pi=pi, r_=r_: (lambda e: [e.dma_start(
                            out=G_[0:64, pi * 128:(pi + 1) * 128].rearrange("p (o c) -> p o c", o=1),
                            in_=sg_s[r_:r_ + 1, n * 128:(n + 1) * 128].partition_broadcast(64))]))(),
                            gbk + "_g", ["sg_s"], [gbk])
                    gbp = G_
                    if br == 0:
                        ts("dve", R_[0:64, :], sbp[0:64, :], eps_t[0:64, 1:2], None, ALU.add, None, [sbk], [rk])
                        recip(R_[0:64, :], R_[0:64, :], [rk], [rk])
                    else:
                        recip(R_[0:64, :], sbp[0:64, :], [sbk], [rk])
                    tt("dve", R_[0:64, :], R_[0:64, :], gbp[0:64, :], ALU.mult, [rk, gbk], [rk])
                    oa, oak = oacc[n % 2], "oacc%d" % (n % 2)
                    if first:
                        tt("pool", oa[0:64, :], X_[0:64, :], R_[0:64, :], ALU.mult, [xk, rk], [oak])
                    else:
                        tt("pool", X_[0:64, :], X_[0:64, :], R_[0:64, :], ALU.mult, [xk, rk], [xk])
                        tt("pool", oa[0:64, :], oa[0:64, :], X_[0:64, :], ALU.add, [oak, xk], [oak])
                    if last:
                        for par in range(2):
                            dma("pool", oT_s[0:2, par * 64:(par + 1) * 64, n * 128:(n + 1) * 128].rearrange("c d q -> d c q"),
                                oa[0:64, par * 256:(par + 1) * 256].rearrange("d (c q) -> d c q", c=2), oak + "_st", [oak], ["oT%d" % (n // 4)])
                else:
                    tt("dve", R_[0:64, :], sbp[0:64, :], esb[0:64, g, :], ALU.add, [sbk, "esb"], [rk])
                    act(R_[0:64, :], R_[0:64, :], AF.Ln, [rk], [rk])
                    act(R_[0:64, :], R_[0:64, :], AF.Exp, [rk], [rk], scale=-1.0)
                    tt("pool", ob[0:64, :], X_[0:64, :], R_[0:64, :], ALU.mult, [xk, rk], [obk])
                    for par in range(2):
                        dma("pool", oT_s[4 + 2 * g:6 + 2 * g, par * 64:(par + 1) * 64, n * 128:(n + 1) * 128].rearrange("c d q -> d c q"),
                            ob[0:64, par * 256:(par + 1) * 256].rearrange("d (c q) -> d c q", c=2), obk + "_st", [obk], ["oT%d" % (n // 4)])

            def attn_units(units, qt, acc, acck):
                nu = len(units)
                es = []
                pend = None
                for ui, (k_ap, kk_, K, qk_, b_ap, bk_, v_ap, vk_) in enumerate(units):
                    stp, stk = next_ps(0, 2)
                    mm(stp[:], k_ap, qt[:, :], True, b_ap is None, kk_ + qk_, [stk])
                    if b_ap is not None:
                        mm(stp[:], ident_bf[:], b_ap, False, True, ["ident_bf"] + bk_, [stk])
                    E_, ek = next_e()
                    act(E_[:], stp[:], AF.Exp, [stk], [ek])
                    es.append((E_, ek))
                    if pend is not None:
                        pu, pE, pek, pv, pvk = pend
                        mm(acc[:, :], pv, pE[:], pu == 0, pu == nu - 1, pvk + [pek], [acck])
                    pend = (ui, E_, ek, v_ap, vk_)
                pu, pE, pek, pv, pvk = pend
                mm(acc[:, :], pv, pE[:], pu == 0, pu == nu - 1, pvk + [pek], [acck])
                return es

            KTs, KTw = slot(1), slot(2)
            VS = slot(4, 2)[:, 0:6144].rearrange("p (t c) -> p t c", c=192)
            VW = slot(6, 2)[:, 0:6144].rearrange("p (t c) -> p t c", c=192)
            dma("sp", KTs[0:64, :], ksT_s[:, :], "ar1", ["ksT_s"], [AR[1]] + WIN)
            dma("sp", KTs[64:128, :], expand_s[:, :], "ar1", ["expand_s"], [AR[1]])
            dma("sp", KTw[0:64, :], kwT_s[:, :], "ar2", ["kwT_s"], [AR[2]] + WIN)
            memset("pool", KTw[64:128, :], 0.0, [AR[2]])
            dma("sp", VS, vsA_s.rearrange("(t p) c -> p t c", p=128), "ar4", ["vsA_s"], [AR[4], AR[5]] + WIN)
            dma("sp", VW, vwA_s.rearrange("(t p) c -> p t c", p=128), "ar6", ["vwA_s"], [AR[6], AR[7], "w1k", "w1v"])

            def load_q(n, src, srckey):
                qt, qtk = qbuf[rot.setdefault("q", 0) % 3], "qbuf%d" % (rot["q"] % 3)
                rot["q"] += 1
                dma("sp", qt[0:64, :], src[:, n, :], qtk, [srckey], [qtk])
                return qt, qtk

            def A1(n):
                qt, qtk = load_q(n, qaT_s, "qaT%d" % (n // 4))
                sgt, sgk_ = sgb[n % 2], "sgb%d" % (n % 2)
                dma("sp", sgt[0:12, :], sg_s[:, n * 128:(n + 1) * 128], sgk_, ["sg_s"], [sgk_])
                units = []
                js = []
                for j in range(2):
                    m = n - 16 * j
                    if m < 0:
                        continue
                    js.append(j)
                    if m < 20:
                        ci = rot["cb"] = (rot.get("cb", 0) + 1) % 2
                        dma("sp", cbt[ci][:], cmpbb_s[m, :, :], "cbt%d" % ci, ["cmpbb"], ["cbt%d" % ci])
                        b_ap, bk_ = cbt[ci][:], ["cbt%d" % ci]
                    else:
                        b_ap, bk_ = None, []
                    units.append((kcT[:, j * 128:(j + 1) * 128], ["kcT"], 128, [qtk, qtk + "n"], b_ap, bk_, vcA[:, j, 0:128], ["vcA"]))
                acc, acck = acc_bank()
                es = attn_units(units, qt, acc, acck)
                ip, ipk = ps[7], PS[7]
                for pi in range(4):
                    for ui, (E_, ek) in enumerate(es):
                        mm(ip[:, pi * 65:(pi + 1) * 65], E_[:, pi * 128:(pi + 1) * 128], ovl1[:, js[ui], :], ui == 0, ui == len(es) - 1,
                           [ek, "ovl1"], [ipk])
                ipv = ip[:, 0:260].rearrange("p (h c) -> p h c", c=65)
                ts("dve", rs4[:], ipv[:, :, 64], eps_t[:, 1:2], None, ALU.add, None, [ipk], ["rs4"])
                recip(rs4[:], rs4[:], ["rs4"], ["rs4"])
                ts("dve", impS[:], ip[:, 0:64], rs4[:, 0:1], None, ALU.mult, None, [ipk, "rs4"], ["impS"])
                for pi in range(1, 4):
                    stt(impS[:], ip[:, pi * 65:pi * 65 + 64], rs4[:, pi:pi + 1], impS[:], ALU.mult, ALU.add, [ipk, "rs4", "impS"], ["impS"])
                u0 = 63 - 2 * n
                tt("dve", imp2[:], impS[:], awbw[:, 0, u0:u0 + 64], ALU.mult, ["impS", "awbw"], ["imp2"])
                tt("dve", imp2[:], imp2[:], awbw[:, 1, u0:u0 + 64], ALU.add, ["imp2", "awbw"], ["imp2"])
                memset("dve", imp2[:, 0:1], BIG, ["imp2"])
                S.op("dve", lambda e: e.max(out=m8[:, 0:8], in_=imp2[:]), ["imp2"], ["m8a"])
                S.op("dve", lambda e: e.match_replace(out=imp3[:], in_to_replace=m8[:, 0:8], in_values=imp2[:], imm_value=-2e9),
                     ["imp2", "m8a"], ["imp3"])
                S.op("dve", lambda e: e.max(out=m8[:, 8:16], in_=imp3[:]), ["imp3"], ["m8b"])
                nt_, ntk = nmts[n % 2], "nmt%d" % (n % 2)
                ts("dve", nt_[:, 64:128], imp2[:], m8[:, 15:16], NEGM, ALU.is_lt, ALU.mult, ["imp2", "m8b"], [ntk])
                return qt, qtk, acc, acck

            def A1b(n, qt, qtk, acc, acck):
                nt_, ntk = nmts[n % 2], "nmt%d" % (n % 2)
                tp, tpk = ps[6], PS[6]
                S.op("pe", lambda e: e.transpose(out=tp[:, 0:128], in_=nt_[:], identity=ident_f[:]), [ntk, "ident_f"], [tpk])
                for pi in range(4):
                    cp("dve" if pi % 2 else "act", qt[64:128, pi * 128:(pi + 1) * 128], tp[64:128, 0:128], [tpk], [qtk + "n"])
                finalize(acc, acck, "nsa", n, br=0, first=True)
                return qt, qtk

            def A23(n, qt, qtk):
                units = []
                for j in range(n + 1):
                    dl = n - j
                    if dl < 5:
                        b_ap, bk_ = slcb[:, dl, :], ["slcb"]
                    else:
                        b_ap, bk_ = None, []
                    units.append((KTs[:, j * 128:(j + 1) * 128], [AR[1]], 128, [qtk, qtk + "n"], b_ap, bk_, VS[:, j, 0:128], [AR[4]]))
                acc_s, acck_s = acc_bank()
                attn_units(units, qt, acc_s, acck_s)
                units = []
                for j in range(max(0, n - 4), n + 1):
                    dl = n - j
                    b_ap = winb4[:] if dl == 4 else slcb[:, dl, :]
                    units.append((KTw[:, j * 128:(j + 1) * 128], [AR[2]], 128, [qtk, qtk + "n"], b_ap, ["winb4", "slcb"], VW[:, j, 0:128], [AR[6]]))
                acc, acck = acc_bank()
                attn_units(units, qt, acc, acck)
                finalize(acc_s, acck_s, "nsa", n, br=1)
                finalize(acc, acck, "nsa", n, br=2, last=True)

            nA = NQB if stop_after != "A4" else 4
            st0 = A1(0)
            pend = A1b(0, *st0)
            for n in range(nA):
                stn = A1(n + 1) if n + 1 < nA else None
                A23(n, *pend)
                pend = A1b(n + 1, *stn) if stn is not None else None
            if stop_after in ("A", "A4"):
                break

            KTc = slot(2)
            VC = slot(4, 2)[:, 0:6144].rearrange("p (t c) -> p t c", c=192)
            dma("sp", KTc[0:64, :], kcT_s[:, :], "ar2", ["kcT_s"], [AR[2]])
            dma("sp", VC, vcA_s.rearrange("(t p) c -> p t c", p=128), "ar4", ["vcA_s"], [AR[4], AR[5]])
            pfin = None
            for n in range(NQB):
                for g in range(2):
                    qt, qtk = load_q(n, qcT_s[g], "qcT%d_%d" % (g, n // 4))
                    units = []
                    for j in range(max(0, n - 1), n + 1):
                        dl = n - j
                        units.append((KTc[:, j * 128:(j + 1) * 128], [AR[2]], 128, [qtk, qtk + "n"], swab[:, g, dl, :], ["swab"],
                                      VC[:, j, 0:128], [AR[4]]))
                    acc, acck = acc_bank()
                    attn_units(units, qt, acc, acck)
                    oi = rot["ost"] = (rot.get("ost", 0) + 1) % 2
                    if pfin is not None:
                        finalize(*pfin[0], **pfin[1])
                    pfin = ((acc, acck, "swa", n), dict(g=g, ob=ost[oi], obk="ost%d" % oi))
            finalize(*pfin[0], **pfin[1])
            if stop_after == "C":
                break

            for h in range(4):
                dma("sp", slot(h)[0:96, :], kbT_s[h, :, :], "ar%d" % h, ["kbT%d" % h], [AR[h]])
                dma("sp", slot(4 + h).rearrange("p (t c) -> p t c", c=128), vbA_s[h].rearrange("(t p) c -> p t c", p=128),
                    "ar%d" % (4 + h), ["vbA%d" % h], [AR[4 + h]])
            for h in range(4):
                KTh = slot(h)
                Vh = slot(4 + h).rearrange("p (t c) -> p t c", c=128)
                par = h % 2
                for J in range(NG):
                    qt, qtk = qbuf[rot["q"] % 3], "qbuf%d" % (rot["q"] % 3)
                    rot["q"] += 1
                    dma("sp", qt[0:96, :], qbT_s[h, :, J * 512:(J + 1) * 512], qtk, ["qbT%d" % h], [qtk])
                    accp, acck = acc_bank()
                    nk = 4 * J + 4
                    pend = None

                    def pv(p):
                        pj, pc0, pE, pek = p
                        mm(accp[:, pc0:512], Vh[:, pj, :], pE[:, pc0:512], pj == 0, pj == nk - 1, [AR[4 + h], pek], [acck])
                    for j in range(nk):
                        r_ = j - 4 * J
                        c0 = 0 if r_ < 0 else r_ * 128
                        stp, stk = next_ps(0, 2)
                        mm(stp[:, c0:512], KTh[0:96, j * 128:(j + 1) * 128], qt[0:96, c0:512], True, r_ < 0, [AR[h], qtk], [stk])
                        if r_ >= 0:
                            mm(stp[:, c0:c0 + 128], ident_bf[:], mlamask[:], False, True, ["ident_bf", "mlamask"], [stk])
                        E_, ek = next_e()
                        act(E_[:, c0:512], stp[:, c0:512], AF.Exp, [stk], [ek])
                        if pend is not None:
                            pv(pend)
                        pend = (j, c0, E_, ek)
                    pv(pend)
                    i2 = rot["fin"] = (rot.get("fin", 0) + 1) % 2
                    X_, xk_ = xe[i2], "xe%d" % i2
                    cp("act" if i2 == 0 else "dve", X_[0:65, :], accp[0:65, :], [acck], [xk_])
                    sbp, sbk = ps[6], PS[6]
                    mm(sbp[:, :], selsum[0:65, 0, :], X_[0:65, :], True, True, ["selsum", xk_], [sbk])
                    R_, rk = rr[i2], "rr%d" % i2
                    recip(R_[0:64, :], sbp[0:64, :], [sbk], [rk])
                    oi = rot["ost"] = (rot.get("ost", 0) + 1) % 2
                    ob, obk = ost[oi], "ost%d" % oi
                    tt("pool", ob[0:64, :], X_[0:64, :], R_[0:64, :], ALU.mult, [xk_, rk], [obk])
                    dma("pool", oT_s[2 + h // 2, par * 64:(par + 1) * 64, J * 512:(J + 1) * 512], ob[0:64, :], obk + "_st", [obk], ["oT%d" % J])
            if stop_after == "B":
                break

            for k in range(8):
                fl, fk = (f32b_flat, "f32b")
                dma("sp", fl[:, 0:D], w_out_d[l, k, :, :], fk, (), [fk])
                cp("dve" if k % 2 == 0 else "pool", wout_bf[:, k, :], fl[:, 0:D], [fk], ["wout"] + AR[0:2])
            obufs = [arena[:, (2 + 2 * i) * 4096:(4 + 2 * i) * 4096].bitcast(F32).rearrange("p (k c) -> p k c", k=8) for i in range(2)]
            okeys = [[AR[2], AR[3]], [AR[4], AR[5]]]
            xbufs = [f32a[:], arena[:, 6 * 4096:8 * 4096].bitcast(F32).rearrange("p (k c) -> p k c", k=8)]
            xkeys = [["f32a"], [AR[6], AR[7]]]
            for G in range(NG):
                tsl = slice(G * 512, (G + 1) * 512)
                YK = "y%d" % G
                hb, hk = bf8s[G % 2], "hT%d_" % (G % 2)
                ob_, obk_ = obufs[G % 2], okeys[G % 2]
                xb_, xbk_ = xbufs[G % 2], xkeys[G % 2]
                hks = [hk + str(k) for k in range(8)]
                dma("sp", ob_, oT_s.rearrange("k p t -> p k t")[:, :, tsl], "obuf%d" % (G % 2), ["oT%d" % G], obk_)
                dma("sp", hb[:], szT_s.rearrange("k p t -> p k t")[:, :, tsl], "hbld%d" % (G % 2), ["szT%d" % G], hks)
                dma("sp", xb_, xsrc.rearrange("k p t -> p k t")[:, :, tsl], "xbuf%d" % (G % 2), [YK], xbk_)
                for k in range(8):
                    tt("dve" if k % 2 == 0 else "pool", hb[:, k, :], ob_[:, k, :], hb[:, k, :], ALU.mult, obk_ + [hk + str(k)], [hk + str(k)])
                ssb, ssk = ps[7], PS[7]
                for fo in range(8):
                    pst, psk = next_ps(0, 6)
                    for k in range(8):
                        mm(pst[:], wout_bf[:, k, fo * 128:(fo + 1) * 128], hb[:, k, :], k == 0, k == 7, ["wout", hk + str(k)], [psk])
                    cp("dve", f32b[:, fo, :], pst[:], [psk], ["Y%d" % fo])
                    q_, qk = sqb[fo % 2], "sqb%d" % (fo % 2)
                    tt("pool", q_[:], f32b[:, fo, :], f32b[:, fo, :], ALU.mult, ["Y%d" % fo], [qk])
                    mm(ssb[:], ones_bf[:], q_[:], fo == 0, fo == 7, ["ones_bf", qk], [ssk])
                act(rstd_t[:], ssb[:], AF.Ln, [ssk], ["rstd"], bias=eps_t[:, 0:1], scale=1.0 / D)
                act(rstd_t[:], rstd_t[:], AF.Exp, ["rstd"], ["rstd"], scale=-0.5)
                for fo in range(8):
                    tt("pool", f32b[:, fo, :], f32b[:, fo, :], rstd_t[:], ALU.mult, ["Y%d" % fo, "rstd"], ["Y%d" % fo])
                    stt(ob_[:, fo, :], f32b[:, fo, :], gpost[:, fo:fo + 1], xb_[:, fo, :], ALU.mult, ALU.add,
                        ["Y%d" % fo, "gpost", "f32b"] + xbk_, obk_)
                o_ = dma("pool", y_d.rearrange("k p t -> p k t")[:, :, tsl], ob_, "obst%d" % (G % 2), obk_, [YK])
                if last_layer:
                    S._final.append(o_)
        S.emit(final_waits=[o for o in S._final if o is not None])
    return nc, S


_NC_CACHE = {}


def _prep_shared(inputs, L):
    c = _const_tables()
    f = lambda a: np.ascontiguousarray(np.asarray(a, np.float32))
    slcb, winb4, swab, cmpb = _bias_tiles(inputs["rel_bias"])
    m = {
        "w_in": f(np.asarray(inputs["w_in"])[:L].reshape(L, 8, 128, DIN)),
        "w_out": f(np.asarray(inputs["w_out"])[:L].reshape(L, 8, 128, D)),
        "gpre": f(np.asarray(inputs["norm_pre"])[:L].reshape(L, 8, 128).transpose(0, 2, 1)),
        "gpost": f(np.asarray(inputs["norm_post"])[:L].reshape(L, 8, 128).transpose(0, 2, 1)),
        "peT": f(np.asarray(inputs["cmp_pos"])[:L].transpose(0, 1, 3, 2).reshape(L, 128, 32)),
        "w1": f(np.asarray(inputs["cmp_w1"])[:L].reshape(L, 2, 32, 64, 128)),
        "w2": f(np.asarray(inputs["cmp_w2"])[:L]),
        "qn": f(np.asarray(inputs["mla_q_norm"])[:L].reshape(L, 2, 128).transpose(0, 2, 1)),
        "wuq": f(np.asarray(inputs["mla_w_uq"])[:L].reshape(L, 2, 128, 384)),
        "kvn": f(np.asarray(inputs["mla_kv_norm"])[:L].reshape(L, 128, 1)),
        "wukv": f(np.asarray(inputs["mla_w_ukv"])[:L]),
        "sinks": f(np.broadcast_to(np.asarray(inputs["swa_sinks"])[:L, None, :], (L, 128, 8))),
        "slcb": f(slcb), "winb4": f(winb4), "swab": f(swab), "cmpb": f(cmpb),
        "ident": c["ident"], "selsum": c["selsum"], "gsel": c["gsel"], "expand": c["expand"],
        "ovl1": c["ovl1"], "awbw": c["awbw"], "mlamask": c["mlamask"], "ropecs": c["ropecs"],
    }
    return m


def kernel(**inputs):
    L = 4
    x = np.asarray(inputs["x"], np.float32)
    B = x.shape[0]
    if "nc" not in _NC_CACHE:
        _NC_CACHE["nc"] = build(L)[0]
    nc = _NC_CACHE["nc"]
    shared = _prep_shared(inputs, L)
    in_maps = []
    for c in range(8):
        b = c % B
        m = dict(shared)
        m["xT"] = np.ascontiguousarray(x[b].T).reshape(8, 128, T)
        in_maps.append(m)
    res = run_bass_kernel_spmd(nc, in_maps, core_ids=list(range(8)))
    out = np.empty((B, T, D), np.float32)
    for b in range(B):
        out[b] = np.asarray(res.results[b]["y"], np.float32).reshape(D, T).T
    return out
```
